# Optimizing a Trainium2 kernel written in Bass

```python
import jax, jax.numpy as jnp
from jax import lax
import numpy as np

D_MODEL = 1024
BATCH = 16
SEQ = 2048
DEPTH = 1

HG_HEADS = 4
HG_KEY_DIM = 128
HG_VAL_DIM = 128
HG_CHUNK = 64
HG_QK_W = HG_HEADS * HG_KEY_DIM
HG_V_W = HG_HEADS * HG_VAL_DIM
ATT_Q_HEADS = 8
ATT_KV_HEADS = 2
ATT_HEAD_DIM = 64
ATT_GROUP = ATT_Q_HEADS // ATT_KV_HEADS
ATT_Q_W = ATT_Q_HEADS * ATT_HEAD_DIM
ATT_KV_W = ATT_KV_HEADS * ATT_HEAD_DIM
WINDOW = 128
ATT_BLOCK = 128
ROPE_THETA = 500000.0
ROPE_DIM = ATT_HEAD_DIM // 4
N_EXPERTS = 32
TOP_K = 4
D_EXPERT = D_MODEL
SWIGLU_ALPHA = 1.702
SWIGLU_LIMIT = 7.0
MOE_BLOCK = 128
EPS = 1e-6
IN_WIDTHS = (HG_QK_W, HG_QK_W, HG_V_W, HG_V_W, ATT_Q_W, ATT_KV_W, ATT_KV_W, D_MODEL, D_MODEL)
IN_WIDTH = HG_QK_W * 2 + HG_V_W * 2 + ATT_Q_W + ATT_KV_W * 2 + D_MODEL * 2

kernel_name = 'hybrid_hgrn2_swa_sink_moe_adaln'


def _split_points():
    pts, acc = [], 0
    for w in IN_WIDTHS[:-1]:
        acc += w
        pts.append(acc)
    return pts


def rms_norm(x, gain):
    xf = x.astype(jnp.float32)
    y = xf * lax.rsqrt(jnp.mean(xf * xf, axis=-1, keepdims=True) + EPS)
    return (y * gain.astype(jnp.float32)).astype(x.dtype)


def partial_rotary(x, positions):
    half = ROPE_DIM // 2
    inv_freq = ROPE_THETA ** (-jnp.arange(half, dtype=jnp.float32) / half)
    ang = positions.astype(jnp.float32)[:, None, :, None] * inv_freq
    cos, sin = jnp.cos(ang), jnp.sin(ang)
    xr = x[..., :ROPE_DIM].astype(jnp.float32)
    x1, x2 = xr[..., :half], xr[..., half:]
    rot = jnp.concatenate([x1 * cos - x2 * sin, x2 * cos + x1 * sin], axis=-1).astype(x.dtype)
    return jnp.concatenate([rot, x[..., ROPE_DIM:]], axis=-1)


def hgrn2_branch(q_pre, f_pre, i_pre, g_pre, lower_bound, out_norm_gain):
    B, S, _ = q_pre.shape
    f32 = jnp.float32

    def heads(t, d):
        return t.reshape(B, S, HG_HEADS, d).transpose(0, 2, 1, 3)

    forget = lower_bound + (1.0 - lower_bound) * jax.nn.sigmoid(f_pre.astype(f32))
    q = heads(jax.nn.silu(q_pre.astype(f32)), HG_KEY_DIM)
    k = heads(1.0 - forget, HG_KEY_DIM)
    log_f = heads(jnp.log(forget), HG_KEY_DIM)
    v = heads(i_pre.astype(f32), HG_VAL_DIM)
    n_chunks = S // HG_CHUNK

    def chunks(t):
        return t.reshape(B, HG_HEADS, n_chunks, HG_CHUNK, t.shape[-1]).transpose(2, 0, 1, 3, 4)

    causal = jnp.tril(jnp.ones((HG_CHUNK, HG_CHUNK), dtype=bool))[:, :, None]

    def step(state, inp):
        qc, kc, vc, lfc = inp
        b = jnp.cumsum(lfc, axis=-2)
        o_inter = jnp.einsum('bhck,bhkv->bhcv', qc * jnp.exp(b), state)
        rel = b[:, :, :, None, :] - b[:, :, None, :, :]
        decay = jnp.exp(jnp.where(causal, rel, -jnp.inf))
        scores = jnp.einsum('bhik,bhjk,bhijk->bhij', qc, kc, decay)
        o = o_inter + jnp.einsum('bhij,bhjv->bhiv', scores, vc)
        b_last = b[:, :, -1:, :]
        new_state = jnp.exp(b_last[:, :, 0, :])[..., None] * state + jnp.einsum(
            'bhjk,bhjv->bhkv', kc * jnp.exp(b_last - b), vc)
        return new_state, o

    state0 = jnp.zeros((B, HG_HEADS, HG_KEY_DIM, HG_VAL_DIM), f32)
    _, o = lax.scan(step, state0, (chunks(q), chunks(k), chunks(v), chunks(log_f)))
    o = o.transpose(1, 2, 0, 3, 4).reshape(B, HG_HEADS, S, HG_VAL_DIM).transpose(0, 2, 1, 3)
    o = rms_norm(o, out_norm_gain).reshape(B, S, HG_V_W) * jax.nn.silu(g_pre.astype(f32))
    return o.astype(q_pre.dtype)


def swa_branch(q_pre, k_pre, v_pre, positions, q_gain, k_gain, sinks):
    B, S, _ = q_pre.shape
    nb = S // ATT_BLOCK
    f32 = jnp.float32
    q = q_pre.reshape(B, S, ATT_Q_HEADS, ATT_HEAD_DIM).transpose(0, 2, 1, 3)
    k = k_pre.reshape(B, S, ATT_KV_HEADS, ATT_HEAD_DIM).transpose(0, 2, 1, 3)
    v = v_pre.reshape(B, S, ATT_KV_HEADS, ATT_HEAD_DIM).transpose(0, 2, 1, 3)
    q = partial_rotary(rms_norm(q, q_gain), positions)
    k = partial_rotary(rms_norm(k, k_gain), positions)
    q = q.reshape(B, ATT_KV_HEADS, ATT_GROUP, nb, ATT_BLOCK, ATT_HEAD_DIM)

    def banded(t):
        tb = t.reshape(B, ATT_KV_HEADS, nb, ATT_BLOCK, ATT_HEAD_DIM)
        prev = jnp.pad(tb, ((0, 0), (0, 0), (1, 0), (0, 0), (0, 0)))[:, :, :-1]
        return jnp.concatenate([prev, tb], axis=3)

    kb, vb = banded(k), banded(v)
    scores = jnp.einsum('bhgnqd,bhnkd->bhgnqk', q, kb).astype(f32) * (ATT_HEAD_DIM ** -0.5)
    blk = jnp.arange(nb)[:, None] * ATT_BLOCK
    q_pos = blk + jnp.arange(ATT_BLOCK)[None, :]
    k_pos = blk - ATT_BLOCK + jnp.arange(2 * ATT_BLOCK)[None, :]
    dist = q_pos[:, :, None] - k_pos[:, None, :]
    mask = (dist >= 0) & (dist < WINDOW) & (k_pos[:, None, :] >= 0)
    scores = jnp.where(mask, scores, -jnp.inf)
    sink = sinks.astype(f32).reshape(1, ATT_KV_HEADS, ATT_GROUP, 1, 1, 1)
    logits = jnp.concatenate([scores, jnp.broadcast_to(sink, scores.shape[:-1] + (1,))], axis=-1)
    probs = jax.nn.softmax(logits, axis=-1)[..., :-1]
    out = jnp.einsum('bhgnqk,bhnkd->bhgnqd', probs.astype(vb.dtype), vb)
    out = out.reshape(B, ATT_Q_HEADS, S, ATT_HEAD_DIM).transpose(0, 2, 1, 3)
    return out.reshape(B, S, ATT_Q_W)


def moe_ffn(h, w_router, b_router, w_up, b_up, w_down, b_down):
    B, S, D = h.shape
    T = B * S
    A = T * TOP_K
    xf = h.reshape(T, D)
    logits = (xf @ w_router + b_router).astype(jnp.float32)
    top_vals, top_idx = lax.top_k(logits, TOP_K)
    gates = jax.nn.softmax(top_vals, axis=-1)
    flat_e = top_idx.reshape(A)
    flat_tok = (jnp.arange(A, dtype=jnp.int32) // TOP_K).astype(jnp.int32)
    flat_w = gates.reshape(A)
    order = jnp.argsort(flat_e)
    sorted_e = flat_e[order]
    counts = jnp.bincount(flat_e, length=N_EXPERTS)
    padded = (counts + MOE_BLOCK - 1) // MOE_BLOCK * MOE_BLOCK
    start = jnp.cumsum(counts) - counts
    pad_end = jnp.cumsum(padded)
    pad_start = pad_end - padded
    dest = pad_start[sorted_e] + jnp.arange(A) - start[sorted_e]
    P = A + N_EXPERTS * MOE_BLOCK
    n_blocks = P // MOE_BLOCK
    tok_buf = jnp.full((P,), T, jnp.int32).at[dest].set(flat_tok[order])
    w_buf = jnp.zeros((P,), jnp.float32).at[dest].set(flat_w[order])
    block_expert = jnp.minimum(
        jnp.searchsorted(pad_end, jnp.arange(n_blocks) * MOE_BLOCK, side='right'), N_EXPERTS - 1)
    x_pad = jnp.concatenate([xf, jnp.zeros((1, D), xf.dtype)], axis=0)
    x_buf = x_pad[tok_buf].reshape(n_blocks, MOE_BLOCK, D)

    def expert_block(args):
        xb, e = args
        hu = xb @ w_up[e] + b_up[e]
        x_glu = jnp.minimum(hu[:, ::2], SWIGLU_LIMIT)
        x_lin = jnp.clip(hu[:, 1::2], -SWIGLU_LIMIT, SWIGLU_LIMIT)
        act = x_glu * jax.nn.sigmoid(SWIGLU_ALPHA * x_glu) * (x_lin + 1.0)
        return act @ w_down[e] + b_down[e]

    y_buf = lax.map(expert_block, (x_buf, block_expert)).reshape(P, D)
    y = jax.ops.segment_sum(y_buf * w_buf[:, None].astype(y_buf.dtype), tok_buf, num_segments=T + 1)[:T]
    return y.reshape(B, S, D)


def setup_inputs(seed: int = 0) -> dict:
    key = jax.random.key(seed)
    ks = jax.random.split(key, 24)
    D = D_MODEL
    f32 = jnp.float32

    def dense(k, shape, fan_in, scale=1.0):
        return jax.random.normal(k, shape, f32) * (scale * fan_in ** -0.5)

    def gain(k, shape):
        return 1.0 + 0.02 * jax.random.normal(k, shape, f32)

    x = jax.random.normal(ks[0], (BATCH, SEQ, D), f32)
    c = jax.random.normal(ks[1], (BATCH, D), f32)
    positions = (jax.random.randint(ks[2], (BATCH, 1), 0, 4096, dtype=jnp.int32)
                 + jnp.arange(SEQ, dtype=jnp.int32)[None, :]).astype(jnp.int32)
    return {
        'x': x,
        'c': c,
        'positions': positions,
        'w_ada': dense(ks[3], (DEPTH, D, 6 * D), D, 0.5),
        'b_ada': 0.02 * jax.random.normal(ks[4], (DEPTH, 6 * D), f32),
        'norm1_gain': gain(ks[5], (DEPTH, D)),
        'w_in': dense(ks[6], (DEPTH, D, IN_WIDTH), D),
        'lower_bound_logits': 0.1 * jax.random.normal(ks[7], (DEPTH + 1, HG_QK_W), f32),
        'hg_norm_gain': gain(ks[8], (DEPTH, HG_VAL_DIM)),
        'w_hg_branch': dense(ks[9], (DEPTH, HG_V_W, D), HG_V_W),
        'q_norm_gain': gain(ks[10], (DEPTH, ATT_HEAD_DIM)),
        'k_norm_gain': gain(ks[11], (DEPTH, ATT_HEAD_DIM)),
        'attn_sinks': jax.random.normal(ks[12], (DEPTH, ATT_Q_HEADS), f32),
        'w_attn_branch': dense(ks[13], (DEPTH, ATT_Q_W, D), ATT_Q_W),
        'w_out': dense(ks[14], (DEPTH, D, D), D),
        'norm2_gain': gain(ks[15], (DEPTH, D)),
        'w_router': dense(ks[16], (DEPTH, D, N_EXPERTS), D),
        'b_router': 0.01 * jax.random.normal(ks[17], (DEPTH, N_EXPERTS), f32),
        'w_up': dense(ks[18], (DEPTH, N_EXPERTS, D, 2 * D_EXPERT), D),
        'b_up': 0.01 * jax.random.normal(ks[19], (DEPTH, N_EXPERTS, 2 * D_EXPERT), f32),
        'w_down': dense(ks[20], (DEPTH, N_EXPERTS, D_EXPERT, D), D_EXPERT),
        'b_down': 0.01 * jax.random.normal(ks[21], (DEPTH, N_EXPERTS, D), f32),
    }


def reference(x, c, positions, w_ada, b_ada, norm1_gain, w_in, lower_bound_logits, hg_norm_gain,
              w_hg_branch, q_norm_gain, k_norm_gain, attn_sinks, w_attn_branch, w_out, norm2_gain,
              w_router, b_router, w_up, b_up, w_down, b_down):
    lower_bounds = jnp.cumsum(jax.nn.softmax(lower_bound_logits.astype(jnp.float32), axis=0), axis=0)
    cond = jax.nn.silu(c)
    split_pts = _split_points()
    for l in range(DEPTH):
        mod = cond @ w_ada[l] + b_ada[l]
        sh1, sc1, g1, sh2, sc2, g2 = jnp.split(mod[:, None, :], 6, axis=-1)
        h = rms_norm(x, norm1_gain[l]) * (1.0 + sc1) + sh1
        proj = h @ w_in[l]
        hq, hf, hi, hg, aq, ak, av, gate_h, gate_a = jnp.split(proj, split_pts, axis=-1)
        y_h = hgrn2_branch(hq, hf, hi, hg, lower_bounds[l], hg_norm_gain[l]) @ w_hg_branch[l]
        y_a = swa_branch(aq, ak, av, positions, q_norm_gain[l], k_norm_gain[l], attn_sinks[l]) @ w_attn_branch[l]
        merged = jax.nn.sigmoid(gate_h) * y_h + jax.nn.sigmoid(gate_a) * y_a
        x = x + g1 * (merged @ w_out[l])
        h2 = rms_norm(x, norm2_gain[l]) * (1.0 + sc2) + sh2
        x = x + g2 * moe_ffn(h2, w_router[l], b_router[l], w_up[l], b_up[l], w_down[l], b_down[l])
    return x
```

```python
import os
import math
import numpy as np
from contextlib import ExitStack
import concourse.bass as bass
import concourse.mybir as mybir
from concourse.bass_utils import run_bass_kernel_spmd

F32 = mybir.dt.float32
BF16 = mybir.dt.bfloat16
I32 = mybir.dt.int32
AF = mybir.ActivationFunctionType
ALU = mybir.AluOpType
AX = mybir.AxisListType

D = 1024
SEQ = 2048
NCORES = 8
NEXP = 32
EPS = 1e-6
PASS_T = 1024
NT = 8
TWO_PI = 2.0 * math.pi


class Buf:
    __slots__ = ("name", "w", "r", "const", "sem", "key", "cnt")

    def __init__(self, name, const=False):
        self.name = name
        self.w = None
        self.r = {}
        self.const = const
        self.sem = None
        self.key = None
        self.cnt = 0


class Eng:
    def __init__(self, name, eng, sem):
        self.name, self.eng, self.sem = name, eng, sem
        self.count = 0
        self.waited = {}


class KB:
    def __init__(self, nc, es):
        self.nc, self.es = nc, es
        self.sems = {}
        self.E = {}
        for nm, eng in [("pe", nc.tensor), ("act", nc.scalar), ("dve", nc.vector),
                        ("pool", nc.gpsimd), ("sp", nc.sync)]:
            sem = es.enter_context(nc.semaphore("s_" + nm))
            self.sems[nm] = sem
            self.E[nm] = Eng(nm, eng, sem)
        self.nd = 0
        self.dma_bufs = []

    def _wait(self, E, tok):
        key, val = tok
        if E.waited.get(key, 0) >= val:
            return
        E.eng.wait_ge(self.sems[key], val)
        E.waited[key] = val

    def _deps(self, E, reads, writes):
        for b in reads:
            if b.w is not None:
                self._wait(E, b.w)
        for b in writes:
            if b.w is not None:
                self._wait(E, b.w)
            for k, v in b.r.items():
                self._wait(E, (k, v))

    def _mark(self, tok, reads, writes):
        for b in writes:
            b.w = tok
            b.r = {}
        for b in reads:
            if not b.const:
                k, v = tok
                if b.r.get(k, 0) < v:
                    b.r[k] = v

    def op(self, en, fn, reads=(), writes=()):
        E = self.E[en]
        self._deps(E, reads, writes)
        ins = fn(E.eng)
        E.count += 1
        ins.then_inc(E.sem, 1)
        tok = (en, E.count)
        self._mark(tok, reads, writes)
        return tok

    def dma(self, out, in_, reads=(), writes=(), sb=None, qn="sp", **kw):
        Q = self.E[qn]
        self._deps(Q, reads, writes)
        if sb.sem is None:
            self.nd += 1
            sb.key = "d%d" % self.nd
            sb.sem = self.es.enter_context(self.nc.semaphore("sd%d" % self.nd))
            self.sems[sb.key] = sb.sem
            self.dma_bufs.append(sb)
        Q.eng.dma_start(out=out, in_=in_, **kw).then_inc(sb.sem, 16)
        sb.cnt += 16
        tok = (sb.key, sb.cnt)
        self._mark(tok, reads, writes)
        return tok

    def barrier(self):
        toks = [(n, e.count) for n, e in self.E.items() if e.count > 0]
        toks += [(b.key, b.cnt) for b in self.dma_bufs]
        for e in self.E.values():
            for t in toks:
                self._wait(e, t)


def full(t):
    return t[tuple(slice(None) for _ in t.shape)]


def build_program(cfg):
    npass = cfg.get("npass", 4)
    nexp = cfg.get("nexp", NEXP)
    dbg = cfg.get("dbg", False)
    stop = cfg.get("stop", "")
    nc = bass.Bass("TRN2", target_bir_lowering=False)

    def din(name, shape, dt=F32):
        return nc.dram_tensor(name, list(shape), dt, kind="ExternalInput").ap()

    def dout(name, shape, dt=F32):
        return nc.dram_tensor(name, list(shape), dt, kind="ExternalOutput").ap()

    x_d = din("x", [2, SEQ, D])
    out_d = dout("out", [2, SEQ, D])
    cT_d = din("cT", [128, 8, 2])
    pos_d = din("posT", [128, 2, 16], I32)
    wada_d = din("w_ada", [D, 6 * D])
    badaT_d = din("b_adaT", [128, 48])
    bg1_d = din("b_g1", [128, D])
    bg2_d = din("b_g2", [128, D])
    n1g_d = din("n1gT", [128, 8])
    n2g_d = din("n2gT", [128, 8])
    win_d = din("w_in", [D, 4864])
    lbl_d = din("lblT", [128, 2, 4])
    hgain_d = din("hgainT", [128, 1])
    whg_d = din("w_hg", [512, D])
    watt_d = din("w_attn", [512, D])
    wout_d = din("w_out", [D, D])
    qg_d = din("qg_bc", [128, 512])
    kg_d = din("kg_bc", [128, 128])
    sink_d = din("sink_bc", [128, 8])
    invf_d = din("invf_bc", [128, 8])
    wr_d = din("w_router", [D, 32])
    br_d = din("br_bc", [128, 32])
    wug_d = din("w_up_g", [nexp, D, D])
    wul_d = din("w_up_l", [nexp, D, D])
    wdn_d = din("w_down", [nexp, D, D])
    bug_d = din("bupgT", [128, NEXP, 8])
    bul_d = din("buplT", [128, NEXP, 8])
    bdn_d = din("b_down", [NEXP, D])
    ident_d = din("ident", [128, 128])
    mbd_d = din("mask_bd", [128, 128])
    mcur_d = din("mask_cur", [128, 128])
    mprev_d = din("mask_prev", [128, 128])
    rmask_d = din("resetmask", [128, PASS_T])
    dbg_d = {}
    if dbg:
        dbg_d["hT"] = dout("dbg_hT", [128, 8, PASS_T], BF16)
        dbg_d["ofinT"] = dout("dbg_ofinT", [128, 4, PASS_T], BF16)
        dbg_d["attnT"] = dout("dbg_attnT", [128, 4, PASS_T], BF16)
        dbg_d["mergedT"] = dout("dbg_mergedT", [128, 8, PASS_T], BF16)
        dbg_d["x1"] = dout("dbg_x1", [128, 8, D], F32)
        dbg_d["h2T"] = dout("dbg_h2T", [128, 8, PASS_T], BF16)
        dbg_d["gates"] = dout("dbg_gates", [128, 8, 32], F32)
        dbg_d["modv"] = dout("dbg_modv", [128, 4, 8, 2], F32)
        dbg_d["g1bc"] = dout("dbg_g1bc", [128, D], F32)

    with ExitStack() as es:
        kb = KB(nc, es)

        uid = [0]

        def sbt(stack, name, shape, dt):
            uid[0] += 1
            return stack.enter_context(nc.sbuf_tensor("sb%d_%s" % (uid[0], name), list(shape), dt))

        PS = [es.enter_context(nc.psum_tensor("ps%d" % i, [128, 1024], F32)) for i in range(4)]
        PB = [[Buf("psb%d_%d" % (i, j)) for j in range(2)] for i in range(4)]

        def bank(i):
            return PS[i // 2][:, (i % 2) * 512:(i % 2) * 512 + 512], PB[i // 2][i % 2]

        def cload(name, dram, shape, dt=F32):
            t = sbt(es, name, shape, dt)
            b = Buf(name, const=True)
            kb.dma(full(t), dram, writes=[b], sb=b)
            return t, b

        ident, identb = cload("ident", ident_d, [128, 128])
        mbd, mbdb = cload("mbd", mbd_d, [128, 128])
        mcur, mcurb = cload("mcur", mcur_d, [128, 128])
        mprev, mprevb = cload("mprev", mprev_d, [128, 128])
        badaT, badaTb = cload("badaT", badaT_d, [128, 48])
        n1g, n1gb = cload("n1g", n1g_d, [128, 8])
        n2g, n2gb = cload("n2g", n2g_d, [128, 8])
        lbl, lblb = cload("lbl", lbl_d, [128, 2, 4])
        hgain, hgainb = cload("hgain", hgain_d, [128, 1])
        kg, kgb = cload("kg", kg_d, [128, 128])
        sink, sinkb = cload("sink", sink_d, [128, 8])
        invf, invfb = cload("invf", invf_d, [128, 8])
        brbc, brbcb = cload("brbc", br_d, [128, 32])
        bug, bugb = cload("bug", bug_d, [128, NEXP, 8])
        bul, bulb = cload("bul", bul_d, [128, NEXP, 8])
        cT, cTb = cload("cT", cT_d, [128, 8, 2])
        posi, posib = cload("posi", pos_d, [128, 2, 16], I32)

        ones_bf = sbt(es, "ones_bf", [128, 128], BF16)
        onesb = Buf("ones", const=True)
        kb.op("dve", lambda e: e.memset(full(ones_bf), 1.0), writes=[onesb])

        condT = sbt(es, "condT", [128, 8, 2], F32)
        condb = Buf("cond", const=True)
        kb.op("act", lambda e: e.activation(out=full(condT), in_=full(cT), func=AF.Silu),
              reads=[cTb], writes=[condb])
        lb = sbt(es, "lb", [128, 4], F32)
        oml = sbt(es, "oml", [128, 4], F32)
        lbb = Buf("lb", const=True)
        kb.op("dve", lambda e: e.tensor_tensor(out=full(lb), in0=lbl[:, 0, :], in1=lbl[:, 1, :], op=ALU.subtract),
              reads=[lblb], writes=[lbb])
        kb.op("act", lambda e: e.activation(out=full(lb), in_=full(lb), func=AF.Sigmoid), reads=[lbb], writes=[lbb])
        omlb = Buf("oml", const=True)
        kb.op("dve", lambda e: e.tensor_scalar(out=full(oml), in0=full(lb), scalar1=-1.0, scalar2=1.0,
                                              op0=ALU.mult, op1=ALU.add), reads=[lbb], writes=[omlb])
        esink = sbt(es, "esink", [128, 8], F32)
        esinkb = Buf("esink", const=True)
        kb.op("act", lambda e: e.activation(out=full(esink), in_=full(sink), func=AF.Exp), reads=[sinkb], writes=[esinkb])
        kb.op("dve", lambda e: e.tensor_scalar(out=full(bul), in0=full(bul), scalar1=1.0, scalar2=None,
                                              op0=ALU.add), reads=[bulb], writes=[bulb])

        modT = sbt(es, "modT", [128, 4, 8, 2], F32)
        modTb = Buf("modT")
        AT = sbt(es, "AT", [128, 2, 2, 8], F32)
        BT = sbt(es, "BT", [128, 2, 2, 8], F32)
        ABb = Buf("AB")
        g1bc = sbt(es, "g1bc", [128, D], F32)
        g2bc = sbt(es, "g2bc", [128, D], F32)
        g1b, g2b = Buf("g1bc"), Buf("g2bc")

        xres = sbt(es, "xres", [128, NT, D], F32)
        xb = [Buf("x%d" % i) for i in range(NT)]
        hT = sbt(es, "hT", [128, 8, PASS_T], BF16)
        hTb = [Buf("hT%d" % i) for i in range(NT)]
        gates = sbt(es, "gates", [128, NT, 32], F32)
        gatesb = [Buf("gates%d" % i) for i in range(NT)]
        Sst = sbt(es, "Sst", [128, 4, 128], F32)
        Sstb = [Buf("S%d" % i) for i in range(4)]
        kT_all = sbt(es, "kT_all", [128, NT + 1, 128], BF16)
        kTb = [Buf("kT%d" % i) for i in range(NT + 1)]
        v_all = sbt(es, "v_all", [128, NT + 1, 2, 65], BF16)
        vab = [Buf("va%d" % i) for i in range(NT + 1)]
        kb.op("pool", lambda e: e.memset(full(v_all), 1.0), writes=vab)
        stg = [sbt(es, "stg%d" % i, [128, 512], F32) for i in range(4)]
        stgb = [Buf("stg%d" % i) for i in range(4)]
        stg_i = [0]
        small = sbt(es, "small", [128, 64], F32)
        smallb = Buf("small")

        def load_cast(dst_ap, dram_ap, dstbufs, eng, mul_ap=None, mul_bufs=()):
            s = stg_i[0] % 4
            stg_i[0] += 1
            w = dst_ap.shape[-1]
            src = stg[s][:, 0:w]
            kb.dma(src, dram_ap, writes=[stgb[s]], sb=stgb[s])
            if mul_ap is not None:
                kb.op(eng, lambda e: e.tensor_tensor(out=dst_ap, in0=src, in1=mul_ap, op=ALU.mult),
                      reads=[stgb[s]] + list(mul_bufs), writes=dstbufs)
            elif eng == "act":
                kb.op(eng, lambda e: e.copy(out=dst_ap, in_=src), reads=[stgb[s]], writes=dstbufs)
            else:
                kb.op(eng, lambda e: e.tensor_copy(out=dst_ap, in_=src), reads=[stgb[s]], writes=dstbufs)

        def dump(name, t, bufs):
            if dbg and name in dbg_d:
                b = Buf("dbg_" + name)
                kb.dma(dbg_d[name], full(t), reads=bufs, sb=b)

        def emit_mod(b, do_fm):
            with ExitStack() as ms:
                wa = [sbt(ms, "wa%d" % i, [128, 8, 512], F32) for i in range(2)]
                wab = [Buf("wa%d" % i) for i in range(2)]
                condrep = sbt(ms, "condrep", [128, 8, 128], F32)
                crb = Buf("condrep")
                kb.op("dve", lambda e: e.tensor_copy(out=full(condrep),
                                                    in_=condT[:, :, b:b + 1].to_broadcast([128, 8, 128])),
                      reads=[condb], writes=[crb])
                fmvec = {0: 0, 1: 1, 3: 2, 4: 3}
                jj = 0
                for j in range(12):
                    vec, half = j // 2, j % 2
                    if vec in fmvec and not do_fm:
                        continue
                    s = jj % 2
                    jj += 1
                    kb.dma(full(wa[s]), wada_d[:, j * 512:(j + 1) * 512].rearrange("(kc p) n -> p kc n", p=128),
                           writes=[wab[s]], sb=wab[s])
                    if vec in fmvec:
                        vi = fmvec[vec]
                        pa, pbuf = bank(0)
                        for cc in range(4):
                            chunk = half * 4 + cc

                            def fn(e, cc=cc):
                                for kc in range(8):
                                    ins = e.matmul(pa[:, cc * 2:cc * 2 + 2], lhsT=wa[s][:, kc, cc * 128:(cc + 1) * 128],
                                                   rhs=condT[:, kc, :], start=(kc == 0), stop=(kc == 7))
                                return ins
                            kb.op("pe", fn, reads=[wab[s], condb], writes=[pbuf])
                            kb.op("dve", lambda e, cc=cc, chunk=chunk: e.tensor_scalar(
                                out=modT[:, vi, chunk, :], in0=pa[:, cc * 2:cc * 2 + 2],
                                scalar1=badaT[:, vec * 8 + chunk:vec * 8 + chunk + 1], scalar2=None, op0=ALU.add),
                                reads=[pbuf, badaTb], writes=[modTb])
                    else:
                        dst, dstb, bsrc = (g1bc, g1b, bg1_d) if vec == 2 else (g2bc, g2b, bg2_d)
                        pa, pbuf = bank(1)

                        def fn(e):
                            for kc in range(8):
                                ins = e.matmul(pa, lhsT=condrep[:, kc, :], rhs=wa[s][:, kc, :],
                                               start=(kc == 0), stop=(kc == 7))
                            return ins
                        kb.op("pe", fn, reads=[wab[s], crb], writes=[pbuf])
                        sidx = stg_i[0] % 4
                        stg_i[0] += 1
                        kb.dma(full(stg[sidx]), bsrc[:, half * 512:(half + 1) * 512], writes=[stgb[sidx]], sb=stgb[sidx])
                        kb.op("dve", lambda e, dst=dst, sidx=sidx: e.tensor_tensor(
                            out=dst[:, half * 512:(half + 1) * 512], in0=pa, in1=full(stg[sidx]), op=ALU.add),
                            reads=[pbuf, stgb[sidx]], writes=[dstb])
                if do_fm:
                    for n, (gt, gtb, shi, sci) in enumerate([(n1g, n1gb, 0, 1), (n2g, n2gb, 2, 3)]):
                        for bb in range(2):
                            kb.op("dve", lambda e, n=n, bb=bb, sci=sci: e.tensor_scalar(
                                out=AT[:, n, bb, :], in0=modT[:, sci, :, bb], scalar1=1.0, scalar2=None, op0=ALU.add),
                                reads=[modTb], writes=[ABb])
                            kb.op("dve", lambda e, n=n, bb=bb, gt=gt: e.tensor_tensor(
                                out=AT[:, n, bb, :], in0=AT[:, n, bb, :], in1=full(gt), op=ALU.mult),
                                reads=[ABb, gtb], writes=[ABb])
                            kb.op("dve", lambda e, n=n, bb=bb, shi=shi: e.tensor_copy(
                                out=BT[:, n, bb, :], in_=modT[:, shi, :, bb]), reads=[modTb], writes=[ABb])
                    dump("modv", modT, [modTb])
                dump("g1bc", g1bc, [g1b])
                kb.barrier()

        def emit_norm(n, b, tmpstack, router=None):
            junk = sbt(tmpstack, "nj%d" % n, [128, D], BF16)
            junkb = Buf("junk")
            xn = [sbt(tmpstack, "xn%d_%d" % (n, i), [128, D], F32) for i in range(2)]
            xnb = [Buf("xn%d" % i) for i in range(2)]
            smb = [Buf("smn0"), Buf("smn1")]

            def stats(i):
                s = i % 2
                ss = small[:, 4 * s + 0:4 * s + 1]
                rt = small[:, 4 * s + 1:4 * s + 2]
                rstd = small[:, 4 * s + 2:4 * s + 3]
                kb.op("act", lambda e: e.activation(out=full(junk), in_=xres[:, i, :], func=AF.Square, accum_out=ss),
                      reads=[xb[i]], writes=[junkb, smb[s]])
                kb.op("act", lambda e: e.activation(out=rt, in_=ss, func=AF.Sqrt, scale=1.0 / D, bias=EPS),
                      reads=[smb[s]], writes=[smb[s]])
                kb.op("dve", lambda e: e.reciprocal(out=rstd, in_=rt), reads=[smb[s]], writes=[smb[s]])
                kb.op("dve", lambda e: e.tensor_scalar(out=full(xn[s]), in0=xres[:, i, :], scalar1=rstd, scalar2=None,
                                                      op0=ALU.mult), reads=[xb[i], smb[s]], writes=[xnb[s]])

            stats(0)
            for i in range(NT):
                s = i % 2
                for hf in range(2):
                    pa, pbuf = bank(4 + 2 * s + hf)

                    def fn(e, hf=hf, pa=pa):
                        for q in range(4):
                            kc = hf * 4 + q
                            ins = e.transpose(out=pa[:, q * 128:(q + 1) * 128], in_=xn[s][:, kc * 128:(kc + 1) * 128],
                                              identity=full(ident))
                        return ins
                    kb.op("pe", fn, reads=[xnb[s], identb], writes=[pbuf])
                    if hf == 0 and i + 1 < NT:
                        stats(i + 1)
                    for q in range(4):
                        kc = hf * 4 + q
                        if router is None:
                            kb.op("act", lambda e, q=q, kc=kc, pa=pa: e.activation(
                                out=hT[:, kc, i * 128:(i + 1) * 128], in_=pa[:, q * 128:(q + 1) * 128], func=AF.Identity,
                                scale=AT[:, n, b, kc:kc + 1], bias=BT[:, n, b, kc:kc + 1]),
                                reads=[pbuf, ABb], writes=[hTb[i]])
                        else:
                            h2f, h2fb = router["h2f"][s], router["h2fb"][s]
                            kb.op("act", lambda e, q=q, kc=kc, pa=pa: e.activation(
                                out=h2f[:, kc, :], in_=pa[:, q * 128:(q + 1) * 128], func=AF.Identity,
                                scale=AT[:, n, b, kc:kc + 1], bias=BT[:, n, b, kc:kc + 1]),
                                reads=[pbuf, ABb], writes=[h2fb])
                            if q == 3:
                                ksl = slice(hf * 4, hf * 4 + 4)
                                kb.op("pool", lambda e, ksl=ksl: e.tensor_copy(out=hT[:, ksl, i * 128:(i + 1) * 128], in_=h2f[:, ksl, :]),
                                      reads=[h2fb], writes=[hTb[i]])
                                kb.op("dve", lambda e, ksl=ksl: e.tensor_tensor(
                                    out=router["h2lo"][s][:, ksl, :], in0=h2f[:, ksl, :], in1=hT[:, ksl, i * 128:(i + 1) * 128],
                                    op=ALU.subtract), reads=[h2fb, hTb[i]], writes=[router["h2lob"][s]])
                if router is not None and i >= 1:
                    router["fn"](i - 1)
            if router is not None:
                router["fn"](NT - 1)

        for p in range(npass):
            b, h = p // 2, p % 2
            t0 = h * PASS_T
            if h == 0:
                emit_mod(b, do_fm=(p == 0))
                for hd in range(4):
                    kb.op("pool", lambda e, hd=hd: e.memset(Sst[:, hd, :], 0.0), writes=[Sstb[hd]])
            for i in range(NT):
                kb.dma(xres[:, i, :], x_d[b, t0 + i * 128:t0 + (i + 1) * 128, :], writes=[xb[i]], sb=xb[i])

            with ExitStack() as s1:
                winb = sbt(s1, "winb", [128, 8, 2048], BF16)
                winbb = Buf("winb")
                ofinT = sbt(s1, "ofinT", [128, 4, PASS_T], BF16)
                ofb = [Buf("of%d" % i) for i in range(4)]
                attnT = sbt(s1, "attnT", [128, 4, PASS_T], BF16)
                atb = [Buf("at%d" % i) for i in range(NT)]

                wsw = sbt(s1, "wsw", [128, 8, 768], BF16)
                wswb = Buf("wsw")

                def win_jobs(dst, dbuf, c0, ncols, d0=0):
                    jobs = []
                    for kc in range(8):
                        c = 0
                        while c < ncols:
                            w = min(512, ncols - c)
                            jobs.append((dst[:, kc, d0 + c:d0 + c + w], win_d[kc * 128:(kc + 1) * 128, c0 + c:c0 + c + w], dbuf))
                            c += w
                    return jobs

                def run_wjobs(jobs, eng="pool"):
                    for (dst, src, dbuf) in jobs:
                        load_cast(dst, src, [dbuf], eng)

                def load_win(c0, ncols):
                    k = 0
                    for kc in range(8):
                        for cg in range(ncols // 512 if ncols >= 512 else 1):
                            w = min(512, ncols)
                            eng = ("pool", "act")[k % 2]
                            k += 1
                            if w == 512:
                                load_cast(winb[:, kc, cg * 512:(cg + 1) * 512],
                                          win_d[kc * 128:(kc + 1) * 128, c0 + cg * 512:c0 + (cg + 1) * 512],
                                          [winbb], eng)
                            else:
                                raise AssertionError

                run_wjobs(win_jobs(winb, winbb, 0, 2048), eng="dve")
                with ExitStack() as sa:
                    emit_norm(0, b, sa)
                    kb.barrier()
                dump("hT", hT, hTb)
                if stop == "A":
                    continue

                run_wjobs(win_jobs(wsw, wswb, 2048, 768))
                with ExitStack() as sh:
                    v_tok = sbt(sh, "v_tok", [128, NT, 512], BF16)
                    vtb = [Buf("vt%d" % i) for i in range(NT)]
                    rmask, rmaskb = None, None
                    rmask = sbt(sh, "rmask", [128, PASS_T], F32)
                    rmaskb = Buf("rmask", const=True)
                    kb.dma(full(rmask), rmask_d, writes=[rmaskb], sb=rmaskb)
                    names = ["sq", "fg", "bc", "kk", "eb", "enb", "kh", "oall", "sgl", "rr"]
                    T = {nm: sbt(sh, "hg_" + nm, [128, PASS_T], F32) for nm in names}
                    TB = {nm: Buf("hg_" + nm) for nm in names}
                    qt = sbt(sh, "hg_qt", [128, PASS_T], BF16)
                    kt = sbt(sh, "hg_kt", [128, PASS_T], BF16)
                    osq = sbt(sh, "hg_osq", [128, PASS_T], BF16)
                    qtb, ktb, osqb = Buf("qt"), Buf("kt"), Buf("osq")
                    ebl = sbt(sh, "hg_ebl", [128, 16], F32)
                    eblb = Buf("ebl")
                    khtok = [sbt(sh, "khtok%d" % i, [128, 2, 128], BF16) for i in range(2)]
                    khtokb = [Buf("khtok%d" % i) for i in range(2)]
                    for i_ in range(2):
                        kb.op("pool", lambda e, i_=i_: e.memset(full(khtok[i_]), 0.0), writes=[khtokb[i_]])
                    scm = [sbt(sh, "scm%d" % i, [128, 128], BF16) for i in range(2)]
                    scmb = [Buf("scm%d" % i) for i in range(2)]
                    sbf = [sbt(sh, "sbf%d" % i, [128, 128], BF16) for i in range(4)]
                    sbfb = [Buf("sbf%d" % i) for i in range(4)]
                    for i in range(NT):
                        pa, pbuf = bank(6 + (i % 2))

                        def fn(e, i=i, pa=pa):
                            for kc in range(8):
                                ins = e.matmul(pa, lhsT=hT[:, kc, i * 128:(i + 1) * 128], rhs=winb[:, kc, 1024:1536],
                                               start=(kc == 0), stop=(kc == 7))
                            return ins
                        kb.op("pe", fn, reads=[hTb[i], winbb], writes=[pbuf])
                        kb.op("act", lambda e, i=i, pa=pa: e.copy(out=v_tok[:, i, :], in_=pa), reads=[pbuf], writes=[vtb[i]])

                    def proj_fm(col0, pi):
                        for tg in range(2):
                            pa, pbuf = bank(pi * 2 + tg)

                            def fn(e, tg=tg, pa=pa):
                                for kc in range(8):
                                    ins = e.matmul(pa, lhsT=winb[:, kc, col0:col0 + 128],
                                                   rhs=hT[:, kc, tg * 512:(tg + 1) * 512], start=(kc == 0), stop=(kc == 7))
                                return ins
                            kb.op("pe", fn, reads=hTb[tg * 4:(tg + 1) * 4] + [winbb], writes=[pbuf])

                    def ew(en, fn, reads, writes):
                        kb.op(en, fn, reads=reads, writes=writes)

                    for hd in range(4):
                        proj_fm(hd * 128, 0)
                        proj_fm(512 + hd * 128, 1)
                        proj_fm(1536 + hd * 128, 2)
                        for tg in range(2):
                            tsl_ = slice(tg * 512, (tg + 1) * 512)
                            ew("act", lambda e: e.activation(out=T["sq"][:, tsl_], in_=PS[0][:, tsl_], func=AF.Silu),
                               [PB[0][tg]], [TB["sq"]])
                            ew("act", lambda e: e.activation(out=T["sgl"][:, tsl_], in_=PS[2][:, tsl_], func=AF.Silu),
                               [PB[2][tg]], [TB["sgl"]])
                            ew("act", lambda e: e.activation(out=T["fg"][:, tsl_], in_=PS[1][:, tsl_], func=AF.Sigmoid),
                               [PB[1][tg]], [TB["fg"]])
                        ew("dve", lambda e, hd=hd: e.tensor_scalar(out=full(T["fg"]), in0=full(T["fg"]),
                                                                  scalar1=oml[:, hd:hd + 1], scalar2=lb[:, hd:hd + 1],
                                                                  op0=ALU.mult, op1=ALU.add),
                           [TB["fg"], omlb, lbb], [TB["fg"]])
                        ew("act", lambda e: e.activation(out=full(T["rr"]), in_=full(T["fg"]), func=AF.Ln),
                           [TB["fg"]], [TB["rr"]])
                        ew("dve", lambda e: e.tensor_tensor_scan(out=full(T["bc"]), data0=full(rmask), data1=full(T["rr"]),
                                                                initial=0.0, op0=ALU.mult, op1=ALU.add),
                           [TB["rr"], rmaskb], [TB["bc"]])
                        ew("dve", lambda e: e.tensor_scalar(out=full(T["kk"]), in0=full(T["fg"]), scalar1=-1.0, scalar2=1.0,
                                                           op0=ALU.mult, op1=ALU.add), [TB["fg"]], [TB["kk"]])
                        ew("act", lambda e: e.activation(out=full(T["eb"]), in_=full(T["bc"]), func=AF.Exp),
                           [TB["bc"]], [TB["eb"]])
                        ew("act", lambda e: e.activation(out=full(T["enb"]), in_=full(T["bc"]), func=AF.Exp, scale=-1.0),
                           [TB["bc"]], [TB["enb"]])
                        ew("dve", lambda e: e.tensor_tensor(out=full(qt), in0=full(T["sq"]), in1=full(T["eb"]), op=ALU.mult),
                           [TB["sq"], TB["eb"]], [qtb])
                        ew("dve", lambda e: e.tensor_tensor(out=full(kt), in0=full(T["kk"]), in1=full(T["enb"]), op=ALU.mult),
                           [TB["kk"], TB["enb"]], [ktb])
                        bc3 = T["bc"].rearrange("p (c t) -> p c t", t=64)
                        ew("act", lambda e: e.activation(out=full(ebl), in_=bc3[:, :, 63], func=AF.Exp),
                           [TB["bc"]], [eblb])
                        ew("dve", lambda e: e.tensor_tensor(out=T["eb"].rearrange("p (c t) -> p c t", t=64),
                                                           in0=bc3[:, :, 63:64].to_broadcast([128, 16, 64]), in1=bc3,
                                                           op=ALU.subtract), [TB["bc"], TB["eb"]], [TB["eb"]])
                        ew("act", lambda e: e.activation(out=full(T["eb"]), in_=full(T["eb"]), func=AF.Exp),
                           [TB["eb"]], [TB["eb"]])
                        ew("dve", lambda e: e.tensor_tensor(out=full(T["kh"]), in0=full(T["kk"]), in1=full(T["eb"]), op=ALU.mult),
                           [TB["kk"], TB["eb"]], [TB["kh"]])
                        def pair_front(i):
                            s = i % 2
                            pX, pXb = bank(6 + s)
                            ptr, psc = pX[:, 0:128], pX[:, 128:256]
                            pd0, pd1 = pX[:, 256:384], pX[:, 384:512]
                            pO, pOb = bank(s)
                            po = pO[:, 0:128]
                            tsl = slice(i * 128, (i + 1) * 128)
                            kb.op("pe", lambda e: e.transpose(out=ptr, in_=T["kh"][:, tsl], identity=full(ident)),
                                  reads=[TB["kh"], identb], writes=[pXb])
                            kb.op("act", lambda e: e.copy(out=khtok[s][0:64, 0, :], in_=ptr[0:64, :]), reads=[pXb], writes=[khtokb[s]])
                            kb.op("act", lambda e: e.copy(out=khtok[s][64:128, 1, :], in_=ptr[64:128, :]), reads=[pXb], writes=[khtokb[s]])
                            kb.op("pe", lambda e: e.matmul(psc, lhsT=kt[:, tsl], rhs=qt[:, tsl], start=True, stop=True),
                                  reads=[ktb, qtb], writes=[pXb])
                            kb.op("dve", lambda e: e.tensor_tensor(out=full(scm[s]), in0=psc, in1=full(mbd), op=ALU.mult),
                                  reads=[pXb, mbdb], writes=[scmb[s]])

                            def fn(e):
                                e.matmul(pd0, lhsT=khtok[s][:, 0, :], rhs=v_tok[:, i, hd * 128:(hd + 1) * 128],
                                         start=True, stop=True)
                                return e.matmul(pd1, lhsT=khtok[s][:, 1, :], rhs=v_tok[:, i, hd * 128:(hd + 1) * 128],
                                                start=True, stop=True)
                            kb.op("pe", fn, reads=[khtokb[s], vtb[i]], writes=[pXb])

                        def pair_back(i):
                            s = i % 2
                            pX, pXb = bank(6 + s)
                            ptr, psc = pX[:, 0:128], pX[:, 128:256]
                            pd0, pd1 = pX[:, 256:384], pX[:, 384:512]
                            pO, pOb = bank(s)
                            po = pO[:, 0:128]
                            tsl = slice(i * 128, (i + 1) * 128)
                            s0, s1_ = sbf[2 * s], sbf[2 * s + 1]
                            Sh = Sst[:, hd, :]
                            kb.op("dve", lambda e: e.tensor_copy(out=full(s0), in_=Sh), reads=[Sstb[hd]], writes=[sbfb[2 * s]])
                            kb.op("dve", lambda e: e.scalar_tensor_tensor(out=Sh, in0=Sh, scalar=ebl[:, 2 * i:2 * i + 1],
                                                                         in1=pd0, op0=ALU.mult, op1=ALU.add),
                                  reads=[Sstb[hd], eblb, pXb], writes=[Sstb[hd]])
                            kb.op("dve", lambda e: e.tensor_copy(out=full(s1_), in_=Sh), reads=[Sstb[hd]], writes=[sbfb[2 * s + 1]])
                            kb.op("dve", lambda e: e.scalar_tensor_tensor(out=Sh, in0=Sh, scalar=ebl[:, 2 * i + 1:2 * i + 2],
                                                                         in1=pd1, op0=ALU.mult, op1=ALU.add),
                                  reads=[Sstb[hd], eblb, pXb], writes=[Sstb[hd]])

                            def fn2(e):
                                e.matmul(po[:, 0:64], lhsT=v_tok[:, i, hd * 128:(hd + 1) * 128], rhs=scm[s][:, 0:64], start=True, stop=False)
                                e.matmul(po[:, 0:64], lhsT=full(s0), rhs=qt[:, i * 128:i * 128 + 64], start=False, stop=True)
                                e.matmul(po[:, 64:128], lhsT=v_tok[:, i, hd * 128:(hd + 1) * 128], rhs=scm[s][:, 64:128], start=True, stop=False)
                                return e.matmul(po[:, 64:128], lhsT=full(s1_), rhs=qt[:, i * 128 + 64:i * 128 + 128],
                                                start=False, stop=True)
                            kb.op("pe", fn2, reads=[vtb[i], scmb[s], sbfb[2 * s], sbfb[2 * s + 1], qtb], writes=[pOb])
                            kb.op("act", lambda e: e.copy(out=T["oall"][:, tsl], in_=po), reads=[pOb], writes=[TB["oall"]])

                        pair_front(0)
                        for i in range(NT):
                            if i + 1 < NT:
                                pair_front(i + 1)
                            pair_back(i)
                        ew("pool", lambda e: e.tensor_tensor(out=full(osq), in0=full(T["oall"]), in1=full(T["oall"]), op=ALU.mult),
                           [TB["oall"]], [osqb])
                        for tg in range(2):
                            pa, pbuf = bank(2 + tg)
                            kb.op("pe", lambda e, tg=tg, pa=pa: e.matmul(pa, lhsT=full(ones_bf), rhs=osq[:, tg * 512:(tg + 1) * 512],
                                                                        start=True, stop=True), reads=[osqb, onesb], writes=[pbuf])
                        for tg in range(2):
                            tsl_ = slice(tg * 512, (tg + 1) * 512)
                            ew("act", lambda e: e.activation(out=T["rr"][:, tsl_], in_=PS[1][:, tsl_], func=AF.Sqrt, scale=1.0 / 128, bias=EPS),
                               [PB[1][tg]], [TB["rr"]])
                        ew("dve", lambda e: e.reciprocal(out=full(T["rr"]), in_=full(T["rr"])), [TB["rr"]], [TB["rr"]])
                        ew("dve", lambda e: e.tensor_tensor(out=full(T["oall"]), in0=full(T["oall"]), in1=full(T["rr"]), op=ALU.mult),
                           [TB["oall"], TB["rr"]], [TB["oall"]])
                        ew("dve", lambda e, hd=hd: e.scalar_tensor_tensor(out=ofinT[:, hd, :], in0=full(T["oall"]),
                                                                         scalar=hgain[:, 0:1], in1=full(T["sgl"]),
                                                                         op0=ALU.mult, op1=ALU.mult),
                           [TB["oall"], TB["sgl"], hgainb], [ofb[hd]])
                    kb.barrier()
                dump("ofinT", ofinT, ofb)
                if stop == "B":
                    continue

                sw_ = s1.enter_context(ExitStack())
                whg = sbt(sw_, "whg", [128, 4, D], BF16)
                wat = sbt(sw_, "wat", [128, 4, D], BF16)
                wo = sbt(sw_, "wo", [128, 8, D], BF16)
                whgb, watb, wob = Buf("whg"), Buf("wat"), Buf("wo")
                cjobs = win_jobs(winb, winbb, 2816, 2048)
                for (wt, wtb, wd_, nk) in ((whg, whgb, whg_d, 4), (wat, watb, watt_d, 4), (wo, wob, wout_d, 8)):
                    for kc in range(nk):
                        for cg in range(2):
                            cjobs.append((wt[:, kc, cg * 512:(cg + 1) * 512],
                                          wd_[kc * 128:(kc + 1) * 128, cg * 512:(cg + 1) * 512], wtb))
                with ExitStack() as sc:
                    cs = sbt(sc, "cs", [128, 2, NT, 8], F32)
                    csb = Buf("cs")
                    ang = sbt(sc, "ang", [128, NT, 8], F32)
                    kq = sbt(sc, "kq", [128, NT, 8], F32)
                    ki = sbt(sc, "ki", [128, NT, 8], I32)
                    posf = sbt(sc, "posf", [128, NT], F32)
                    tmpm = sbt(sc, "tmpm", [128, NT, 8], F32)
                    angb = Buf("ang")
                    kb.op("dve", lambda e: e.tensor_copy(out=full(posf), in_=posi[:, b, h * NT:(h + 1) * NT]),
                          reads=[posib], writes=[angb])
                    kb.op("dve", lambda e: e.tensor_tensor(out=full(ang), in0=full(posf).unsqueeze(2).to_broadcast([128, NT, 8]),
                                                          in1=full(invf).unsqueeze(1).to_broadcast([128, NT, 8]), op=ALU.mult),
                          reads=[angb, invfb], writes=[angb])
                    C1 = 6.28125
                    C2 = TWO_PI - C1
                    for which, shift in ((0, 0.0), (1, math.pi / 2)):
                        dst = cs[:, which, :, :]
                        A = lambda fn: kb.op("dve", fn, reads=[angb], writes=[angb])
                        A(lambda e: e.tensor_scalar(out=full(kq), in0=full(ang), scalar1=shift, scalar2=1.0 / TWO_PI,
                                                    op0=ALU.add, op1=ALU.mult))
                        A(lambda e: e.tensor_copy(out=full(ki), in_=full(kq)))
                        A(lambda e: e.tensor_copy(out=full(kq), in_=full(ki)))
                        A(lambda e: e.scalar_tensor_tensor(out=full(tmpm), in0=full(kq), scalar=-C1, in1=full(ang),
                                                           op0=ALU.mult, op1=ALU.add))
                        A(lambda e: e.scalar_tensor_tensor(out=full(tmpm), in0=full(kq), scalar=-C2, in1=full(tmpm),
                                                           op0=ALU.mult, op1=ALU.add))
                        if shift != 0.0:
                            A(lambda e: e.tensor_scalar(out=full(tmpm), in0=full(tmpm), scalar1=shift, scalar2=None, op0=ALU.add))
                        A(lambda e: e.tensor_scalar(out=full(kq), in0=full(tmpm), scalar1=math.pi, scalar2=-TWO_PI,
                                                    op0=ALU.is_gt, op1=ALU.mult))
                        A(lambda e: e.tensor_tensor(out=full(tmpm), in0=full(tmpm), in1=full(kq), op=ALU.add))
                        A(lambda e: e.tensor_scalar(out=full(kq), in0=full(tmpm), scalar1=-math.pi, scalar2=TWO_PI,
                                                    op0=ALU.is_lt, op1=ALU.mult))
                        A(lambda e: e.tensor_tensor(out=full(tmpm), in0=full(tmpm), in1=full(kq), op=ALU.add))
                        A(lambda e: e.tensor_scalar(out=full(tmpm), in0=full(tmpm), scalar1=-math.pi, scalar2=math.pi,
                                                    op0=ALU.max, op1=ALU.min))
                        kb.op("act", lambda e, dst=dst: e.activation(out=dst, in_=full(tmpm), func=AF.Sin),
                              reads=[angb], writes=[csb])

                    qg = sbt(sc, "qg", [128, 512], F32)
                    qgb = Buf("qg")
                    kb.dma(full(qg), qg_d, writes=[qgb], sb=qgb)
                    kb.op("dve", lambda e: e.tensor_scalar(out=full(qg), in0=full(qg), scalar1=0.125, scalar2=None,
                                                          op0=ALU.mult), reads=[qgb], writes=[qgb])
                    sqq = sbt(sc, "sqq", [128, 512], F32)
                    qgf = sbt(sc, "qgf", [128, 8, 64], F32)
                    qrot = sbt(sc, "qrot", [128, 4, 2, 64], F32)
                    kgf = sbt(sc, "kgf", [128, 2, 64], F32)
                    krot = sbt(sc, "krot", [128, 2, 64], F32)
                    rt1 = sbt(sc, "rt1", [128, 8, 8], F32)
                    rt2 = sbt(sc, "rt2", [128, 8, 8], F32)
                    rt3 = sbt(sc, "rt3", [128, 8, 8], F32)
                    rt4 = sbt(sc, "rt4", [128, 8, 8], F32)
                    sm2 = sbt(sc, "sm2", [128, 32], F32)
                    qT_all = [sbt(sc, "qT_all%d" % j, [128, 4, 128], BF16) for j in range(2)]
                    sm3 = sbt(sc, "sm3", [128, 8], F32)
                    sm3b = Buf("sm3")
                    Pm = [sbt(sc, "Pm%d" % i, [128, 4, 128], BF16) for i in range(4)]
                    Pe = [sbt(sc, "Pe%d" % i, [128, 4, 128], BF16) for i in range(4)]
                    attn_tok = sbt(sc, "attn_tok", [128, 8, 64], F32)
                    qwb, kwb, sm2b, qTb = Buf("qw"), Buf("kw"), Buf("sm2"), [Buf("qT0"), Buf("qT1")]
                    Pmb = [Buf("Pm%d" % i) for i in range(4)]
                    Peb = [Buf("Pe%d" % i) for i in range(4)]
                    atokb = Buf("atok")

                    def normrot(src3, nh, gain_t, gainb_, gf, wbuf, srcbuf, i):
                        Q = lambda en, fn, extra=(): kb.op(en, fn, reads=[wbuf, srcbuf, sm2b] + list(extra), writes=[wbuf, sm2b])
                        sq3 = sqq[:, 0:nh * 64].rearrange("p (h d) -> p h d", d=64)
                        Q("act", lambda e: e.activation(out=sq3, in_=src3, func=AF.Square))
                        Q("dve", lambda e: e.tensor_reduce(out=sm2[:, 0:nh], in_=sq3, axis=AX.X, op=ALU.add))
                        Q("act", lambda e: e.activation(out=sm2[:, 8:8 + nh], in_=sm2[:, 0:nh], func=AF.Sqrt, scale=1.0 / 64, bias=EPS))
                        Q("dve", lambda e: e.reciprocal(out=sm2[:, 16:16 + nh], in_=sm2[:, 8:8 + nh]))
                        Q("dve", lambda e: e.tensor_tensor(out=full(gf), in0=src3,
                                                          in1=sm2[:, 16:16 + nh].unsqueeze(2).to_broadcast([128, nh, 64]),
                                                          op=ALU.mult))
                        Q("dve", lambda e: e.tensor_tensor(out=full(gf), in0=full(gf),
                                                          in1=gain_t.rearrange("p (h d) -> p h d", d=64), op=ALU.mult), [gainb_])
                        cosb = cs[:, 1, i, :].unsqueeze(1).to_broadcast([128, nh, 8])
                        sinb = cs[:, 0, i, :].unsqueeze(1).to_broadcast([128, nh, 8])
                        x1, x2 = gf[:, :, 0:8], gf[:, :, 8:16]
                        a, bb_ = rt1[:, 0:nh, :], rt2[:, 0:nh, :]
                        c_, d_ = rt3[:, 0:nh, :], rt4[:, 0:nh, :]
                        Q("dve", lambda e: e.tensor_tensor(out=a, in0=x1, in1=cosb, op=ALU.mult), [csb])
                        Q("dve", lambda e: e.tensor_tensor(out=bb_, in0=x2, in1=sinb, op=ALU.mult), [csb])
                        Q("dve", lambda e: e.tensor_tensor(out=c_, in0=x2, in1=cosb, op=ALU.mult), [csb])
                        Q("dve", lambda e: e.tensor_tensor(out=d_, in0=x1, in1=sinb, op=ALU.mult), [csb])
                        Q("dve", lambda e: e.tensor_tensor(out=x1, in0=a, in1=bb_, op=ALU.subtract))
                        Q("dve", lambda e: e.tensor_tensor(out=x2, in0=c_, in1=d_, op=ALU.add))

                    def swa_front(i):
                        slot = i + 1
                        pq_, pqb = bank(0)
                        pkv_, pkvb = bank(1)

                        def fn(e):
                            for kc in range(8):
                                ins = e.matmul(pq_, lhsT=hT[:, kc, i * 128:(i + 1) * 128], rhs=wsw[:, kc, 0:512],
                                               start=(kc == 0), stop=(kc == 7))
                            return ins
                        kb.op("pe", fn, reads=[hTb[i], wswb], writes=[pqb])

                        def fn(e):
                            for kc in range(8):
                                ins = e.matmul(pkv_[:, 0:256], lhsT=hT[:, kc, i * 128:(i + 1) * 128], rhs=wsw[:, kc, 512:768],
                                               start=(kc == 0), stop=(kc == 7))
                            return ins
                        kb.op("pe", fn, reads=[hTb[i], wswb], writes=[pkvb])
                        normrot(pq_.rearrange("p (h d) -> p h d", d=64), 8, qg, qgb, qgf, qwb, pqb, i)
                        kb.op("dve", lambda e: e.tensor_copy(out=qrot.rearrange("p g k d -> p k g d"),
                                                            in_=qgf.rearrange("p (k g) d -> p k g d", k=2)),
                              reads=[qwb], writes=[qwb])
                        normrot(pkv_[:, 0:128].rearrange("p (h d) -> p h d", d=64), 2, kg, kgb, kgf, kwb, pkvb, i)
                        kb.op("act", lambda e: e.copy(out=v_all[:, slot, :, 0:64],
                                                      in_=pkv_[:, 128:256].rearrange("p (h d) -> p h d", d=64)),
                              reads=[pkvb], writes=[vab[slot]])
                        ptq, ptqb = bank(2)
                        ptk, ptkb = bank(3)

                        def fn(e):
                            for g in range(4):
                                ins = e.transpose(out=ptq[:, g * 128:(g + 1) * 128],
                                                  in_=qrot[:, g, :, :].rearrange("p k d -> p (k d)"), identity=full(ident))
                            return ins
                        kb.op("pe", fn, reads=[qwb, identb], writes=[ptqb])
                        kb.op("act", lambda e: e.copy(out=full(qT_all[i % 2]), in_=ptq.rearrange("p (g t) -> p g t", t=128)),
                              reads=[ptqb], writes=[qTb[i % 2]])
                        kb.op("pe", lambda e: e.transpose(out=ptk[:, 0:128], in_=kgf.rearrange("p k d -> p (k d)"),
                                                          identity=full(ident)), reads=[kwb, identb], writes=[ptkb])
                        kb.op("act", lambda e: e.copy(out=kT_all[:, slot, :], in_=ptk[:, 0:128]), reads=[ptkb], writes=[kTb[slot]])
                    def swa_back(i):
                        slot = i + 1
                        blks = [("cur", slot)]
                        if not (h == 0 and i == 0):
                            blks = [("prev", slot - 1), ("cur", slot)]
                        plist = {}
                        for kvh in range(2):
                            for bi_, (bn, bs) in enumerate(blks):
                                pidx = 2 * kvh + bi_
                                psT, psTb = bank(4 + pidx)
                                kb.op("pe", lambda e, kvh=kvh, bs=bs, psT=psT: e.matmul(
                                    psT, lhsT=kT_all[kvh * 64:(kvh + 1) * 64, bs, :],
                                    rhs=qT_all[i % 2][kvh * 64:(kvh + 1) * 64, :, :].rearrange("p g t -> p (g t)"),
                                    start=True, stop=True), reads=[kTb[bs], qTb[i % 2]], writes=[psTb])
                                kb.op("act", lambda e, pidx=pidx, psT=psT: e.activation(
                                    out=Pe[pidx].rearrange("p g t -> p (g t)"), in_=psT, func=AF.Exp),
                                    reads=[psTb], writes=[Peb[pidx]])
                                mk, mkb = (mcur, mcurb) if bn == "cur" else (mprev, mprevb)
                                kb.op("pool", lambda e, pidx=pidx, mk=mk: e.tensor_tensor(
                                    out=full(Pm[pidx]), in0=full(Pe[pidx]),
                                    in1=full(mk).unsqueeze(1).to_broadcast([128, 4, 128]), op=ALU.mult),
                                    reads=[Peb[pidx], mkb], writes=[Pmb[pidx]])
                                plist[(kvh, bn)] = (pidx, bs)
                        for kvh in range(2):
                            pav, pavb = bank(4 + 2 * kvh)
                            pav3 = pav[:, 0:260].rearrange("p (g d) -> p g d", d=65)

                            def fn(e, kvh=kvh, pav3=pav3):
                                ins = None
                                for g in range(4):
                                    for bi, (bn, bs) in enumerate(blks):
                                        pi_, _ = plist[(kvh, bn)]
                                        ins = e.matmul(pav3[:, g, :], lhsT=Pm[pi_][:, g, :], rhs=v_all[:, bs, kvh, :],
                                                       start=(bi == 0), stop=(bi == len(blks) - 1))
                                return ins
                            rd = [Pmb[plist[(kvh, bn)][0]] for (bn, bs) in blks] + [vab[bs] for (bn, bs) in blks]
                            kb.op("pe", fn, reads=rd, writes=[pavb])
                            kb.op("dve", lambda e, kvh=kvh, pav3=pav3: e.tensor_tensor(
                                out=sm3[:, 0:4], in0=pav3[:, :, 64], in1=esink[:, kvh * 4:(kvh + 1) * 4], op=ALU.add),
                                reads=[pavb, esinkb, sm3b], writes=[sm3b])
                            kb.op("dve", lambda e: e.reciprocal(out=sm3[:, 4:8], in_=sm3[:, 0:4]), reads=[sm3b], writes=[sm3b])
                            kb.op("dve", lambda e, kvh=kvh, pav3=pav3: e.tensor_tensor(
                                out=attn_tok[:, kvh * 4:(kvh + 1) * 4, :], in0=pav3[:, :, 0:64],
                                in1=sm3[:, 4:8].unsqueeze(2).to_broadcast([128, 4, 64]), op=ALU.mult),
                                reads=[pavb, sm3b], writes=[atokb])
                        pta, ptab = bank(5)

                        def fn(e):
                            for c in range(4):
                                ins = e.transpose(out=pta[:, c * 128:(c + 1) * 128],
                                                  in_=attn_tok.rearrange("p h d -> p (h d)")[:, c * 128:(c + 1) * 128],
                                                  identity=full(ident))
                            return ins
                        kb.op("pe", fn, reads=[atokb, identb], writes=[ptab])
                        kb.op("act", lambda e: e.copy(out=attnT[:, :, i * 128:(i + 1) * 128],
                                                      in_=pta.rearrange("p (c t) -> p c t", t=128)),
                              reads=[ptab], writes=[atb[i]])
                        run_wjobs(cjobs[i * 8:(i + 1) * 8], eng="dve")
                    swa_front(0)
                    for i in range(NT):
                        if i + 1 < NT:
                            swa_front(i + 1)
                        swa_back(i)
                    kb.op("pool", lambda e: e.tensor_copy(out=kT_all[:, 0, :], in_=kT_all[:, NT, :]), reads=[kTb[NT]], writes=[kTb[0]])
                    kb.op("pool", lambda e: e.tensor_copy(out=v_all[:, 0, :, :], in_=v_all[:, NT, :, :]), reads=[vab[NT]], writes=[vab[0]])
                    kb.barrier()
                dump("attnT", attnT, atb)
                if stop == "C":
                    continue

                with ExitStack() as sd:
                    mergedT = sbt(sd, "mergedT", [128, 8, PASS_T], BF16)
                    mgb = [Buf("mg%d" % i) for i in range(NT)]
                    tmpd = [sbt(sd, "tmpd%d" % i, [128, 512], F32) for i in range(4)]
                    tmpdb = [Buf("tmpd%d" % i) for i in range(4)]
                    for oc in range(8):
                        for tg in range(2):
                            trd = hTb[tg * 4:(tg + 1) * 4]
                            pgh, pghb = bank(0)
                            pga, pgab = bank(1)
                            pyh, pyhb = bank(2)
                            pya, pyab = bank(3)
                            tsl = slice(tg * 512, (tg + 1) * 512)

                            def mmf(pa, wt, col0, src, nk):
                                def fn(e):
                                    for kc in range(nk):
                                        ins = e.matmul(pa, lhsT=wt[:, kc, col0:col0 + 128], rhs=src[:, kc, tsl],
                                                       start=(kc == 0), stop=(kc == nk - 1))
                                    return ins
                                return fn
                            kb.op("pe", mmf(pgh, winb, oc * 128, hT, 8), reads=trd + [winbb], writes=[pghb])
                            kb.op("pe", mmf(pga, winb, 1024 + oc * 128, hT, 8), reads=trd + [winbb], writes=[pgab])
                            kb.op("pe", mmf(pyh, whg, oc * 128, ofinT, 4), reads=ofb + [whgb], writes=[pyhb])
                            kb.op("pe", mmf(pya, wat, oc * 128, attnT, 4), reads=atb[tg * 4:(tg + 1) * 4] + [watb], writes=[pyab])
                            kb.op("act", lambda e: e.activation(out=full(tmpd[0]), in_=pgh, func=AF.Sigmoid), reads=[pghb], writes=[tmpdb[0]])
                            kb.op("act", lambda e: e.activation(out=full(tmpd[1]), in_=pga, func=AF.Sigmoid), reads=[pgab], writes=[tmpdb[1]])
                            kb.op("dve", lambda e: e.tensor_tensor(out=full(tmpd[2]), in0=pyh, in1=full(tmpd[0]), op=ALU.mult),
                                  reads=[pyhb, tmpdb[0]], writes=[tmpdb[2]])
                            kb.op("dve", lambda e: e.tensor_tensor(out=full(tmpd[3]), in0=pya, in1=full(tmpd[1]), op=ALU.mult),
                                  reads=[pyab, tmpdb[1]], writes=[tmpdb[3]])
                            kb.op("pool", lambda e: e.tensor_tensor(out=mergedT[:, oc, tsl], in0=full(tmpd[2]), in1=full(tmpd[3]), op=ALU.add),
                                  reads=[tmpdb[2], tmpdb[3]], writes=mgb[tg * 4:(tg + 1) * 4])
                    dump("mergedT", mergedT, mgb)
                    for i in range(NT):
                        for hf in range(2):
                            pa, pbuf = bank(4 + (2 * i + hf) % 4)

                            def fn(e, hf=hf, pa=pa):
                                for kc in range(8):
                                    ins = e.matmul(pa, lhsT=mergedT[:, kc, i * 128:(i + 1) * 128], rhs=wo[:, kc, hf * 512:(hf + 1) * 512],
                                                   start=(kc == 0), stop=(kc == 7))
                                return ins
                            kb.op("pe", fn, reads=[mgb[i], wob], writes=[pbuf])
                            tt = tmpd[hf]
                            kb.op("dve", lambda e, hf=hf, pa=pa, tt=tt: e.tensor_tensor(
                                out=full(tt), in0=pa, in1=g1bc[:, hf * 512:(hf + 1) * 512], op=ALU.mult),
                                reads=[pbuf, g1b], writes=[tmpdb[hf]])
                            kb.op("pool", lambda e, hf=hf, tt=tt: e.tensor_tensor(
                                out=xres[:, i, hf * 512:(hf + 1) * 512], in0=xres[:, i, hf * 512:(hf + 1) * 512],
                                in1=full(tt), op=ALU.add), reads=[tmpdb[hf], xb[i]], writes=[xb[i]])
                    dump("x1", xres, xb)
                    kb.barrier()
                if stop == "D":
                    continue
                with ExitStack() as sr:
                    h2f = [sbt(sr, "h2f%d" % j, [128, 8, 128], F32) for j in range(2)]
                    h2fb = [Buf("h2f%d" % j) for j in range(2)]
                    h2lo = [sbt(sr, "h2lo%d" % j, [128, 8, 128], BF16) for j in range(2)]
                    h2lob = [Buf("h2lo%d" % j) for j in range(2)]
                    lgs = [sbt(sr, "lg%d" % j, [128, 32], F32) for j in range(2)]
                    exs = [sbt(sr, "ex%d" % j, [128, 32], F32) for j in range(2)]
                    mks = [sbt(sr, "mk%d" % j, [128, 32], F32) for j in range(2)]
                    top8s = [sbt(sr, "top8%d" % j, [128, 8], F32) for j in range(2)]
                    gTs = [sbt(sr, "gT%d" % j, [128, 128], BF16) for j in range(2)]
                    gpads = [sbt(sr, "gpad%d" % j, [128, 128], F32) for j in range(2)]
                    gpadbs = [Buf("gpad%d" % j) for j in range(2)]
                    for j in range(2):
                        kb.op("pool", lambda e, j=j: e.memset(full(gpads[j]), 0.0), writes=[gpadbs[j]])
                    rbs = [Buf("router%d" % j) for j in range(2)]
                    gTbs = [Buf("gT%d" % j) for j in range(2)]
                    wr = sbt(sr, "wr", [128, 8, 32], F32)
                    wrb = Buf("wr")
                    kb.dma(full(wr), wr_d.rearrange("(kc p) n -> p kc n", p=128), writes=[wrb], sb=wrb)
                    wr_hi = sbt(sr, "wr_hi", [128, 8, 32], BF16)
                    wr_lo = sbt(sr, "wr_lo", [128, 8, 32], BF16)
                    wrsb = Buf("wrs")
                    kb.op("dve", lambda e: e.tensor_copy(out=full(wr_hi), in_=full(wr)), reads=[wrb], writes=[wrsb])
                    kb.op("dve", lambda e: e.tensor_tensor(out=full(wr_lo), in0=full(wr), in1=full(wr_hi), op=ALU.subtract),
                          reads=[wrb, wrsb], writes=[wrsb])
                    bdg = sbt(sr, "bdg", [128, D], F32)
                    bdgb = Buf("bdg")
                    kb.op("pool", lambda e: e.memset(full(bdg), 0.0), writes=[bdgb])
                    kb.dma(bdg[0:32, :], bdn_d, writes=[bdgb], sb=bdgb)
                    kb.op("dve", lambda e: e.tensor_tensor(out=full(bdg), in0=full(bdg), in1=full(g2bc), op=ALU.mult),
                          reads=[bdgb, g2b], writes=[bdgb])
                    bdgh = sbt(sr, "bdgh", [128, D], BF16)
                    kb.op("dve", lambda e: e.tensor_copy(out=full(bdgh), in_=full(bdg)), reads=[bdgb], writes=[bdgb])

                    def router_tile(i):
                        par = i % 2
                        h2lo_, h2lob_ = h2lo[par], h2lob[par]
                        lg, ex, mk_, top8, gT, gpad = lgs[par], exs[par], mks[par], top8s[par], gTs[par], gpads[par]
                        rb, gTb, gpadb = rbs[par], gTbs[par], gpadbs[par]
                        sm = small[:, 8 + 4 * par:12 + 4 * par]
                        pl, plb = bank(2 * par)
                        plg = pl[:, 0:32]

                        def fn(e):
                            trip = []
                            for kc in range(8):
                                trip += [(hT[:, kc, i * 128:(i + 1) * 128], wr_hi[:, kc, :]),
                                         (hT[:, kc, i * 128:(i + 1) * 128], wr_lo[:, kc, :]),
                                         (h2lo_[:, kc, :], wr_hi[:, kc, :])]
                            for j, (l_, r_) in enumerate(trip):
                                ins = e.matmul(plg, lhsT=l_, rhs=r_, start=(j == 0), stop=(j == len(trip) - 1))
                            return ins
                        kb.op("pe", fn, reads=[h2lob_, hTb[i], wrsb], writes=[plb])
                        R = lambda en, fn, extra=(), w=(): kb.op(en, fn, reads=[rb] + list(extra), writes=[rb] + list(w))
                        if os.environ.get("KNOGATE"):
                            kb.op("dve", lambda e: e.tensor_copy(out=gates[:, i, :], in_=plg), reads=[plb], writes=[gatesb[i]])
                            return
                        R("dve", lambda e: e.tensor_tensor(out=full(lg), in0=plg, in1=full(brbc), op=ALU.add), [plb, brbcb])
                        R("dve", lambda e: e.max(out=full(top8), in_=full(lg)))
                        R("dve", lambda e: e.tensor_scalar(out=sm[:, 0:1], in0=top8[:, 0:1], scalar1=-1.0, scalar2=None, op0=ALU.mult))
                        R("act", lambda e: e.activation(out=full(ex), in_=full(lg), func=AF.Exp, bias=sm[:, 0:1]))
                        R("dve", lambda e: e.tensor_scalar(out=full(mk_), in0=full(lg), scalar1=top8[:, 3:4], scalar2=None, op0=ALU.is_ge))
                        R("dve", lambda e: e.tensor_tensor(out=full(ex), in0=full(ex), in1=full(mk_), op=ALU.mult))
                        R("dve", lambda e: e.tensor_reduce(out=sm[:, 1:2], in_=full(ex), axis=AX.X, op=ALU.add))
                        R("dve", lambda e: e.reciprocal(out=sm[:, 2:3], in_=sm[:, 1:2]))
                        R("dve", lambda e: e.tensor_scalar(out=gates[:, i, :], in0=full(ex), scalar1=sm[:, 2:3], scalar2=None, op0=ALU.mult),
                          [], [gatesb[i]])
                        if os.environ.get("KNOBIAS"):
                            return
                        pt, ptb = pl, plb
                        kb.op("dve", lambda e: e.tensor_copy(out=gpad[:, 0:32], in_=gates[:, i, :]), reads=[gatesb[i]], writes=[gpadb])
                        kb.op("pe", lambda e: e.transpose(out=pt[:, 128:256], in_=full(gpad), identity=full(ident)),
                              reads=[gpadb, identb], writes=[ptb])
                        kb.op("act", lambda e: e.copy(out=full(gT), in_=pt[:, 128:256]), reads=[ptb], writes=[gTb])
                        for hf in range(2):
                            pb_, pbb = bank(2 * par + 1)
                            kb.op("pe", lambda e, hf=hf, pb_=pb_: e.matmul(pb_, lhsT=full(gT), rhs=bdgh[:, hf * 512:(hf + 1) * 512],
                                                                          start=True, stop=True), reads=[gTb, bdgb], writes=[pbb])
                            kb.op("dve", lambda e, hf=hf, pb_=pb_: e.tensor_tensor(
                                out=xres[:, i, hf * 512:(hf + 1) * 512], in0=pb_, in1=xres[:, i, hf * 512:(hf + 1) * 512], op=ALU.add),
                                reads=[pbb, xb[i]], writes=[xb[i]])
                    KR = os.environ.get("KR", "")
                    if KR == "1":
                        emit_norm(1, b, sr, router=None)
                    elif KR == "2":
                        emit_norm(1, b, sr, router=dict(h2f=h2f, h2fb=h2fb, h2lo=h2lo, h2lob=h2lob, fn=lambda i: None))
                    else:
                        emit_norm(1, b, sr, router=dict(h2f=h2f, h2fb=h2fb, h2lo=h2lo, h2lob=h2lob, fn=router_tile))
                    kb.barrier()
                dump("h2T", hT, hTb)
                dump("gates", gates, gatesb)
            if stop == "R":
                continue

            with ExitStack() as sm:
                wg = [sbt(sm, "wg%d" % i, [128, 8, D], BF16) for i in range(2)]
                wl = [sbt(sm, "wl%d" % i, [128, 8, D], BF16) for i in range(2)]
                wd = [sbt(sm, "wd%d" % i, [128, 8, D], BF16) for i in range(2)]
                wgb = [Buf("wg%d" % i) for i in range(2)]
                wlb = [Buf("wl%d" % i) for i in range(2)]
                wdb = [Buf("wd%d" % i) for i in range(2)]
                act = [sbt(sm, "act%d" % i, [128, 8, 512], BF16) for i in range(2)]
                actb = [Buf("act%d" % i) for i in range(2)]
                tg_ = [sbt(sm, "tg%d" % i, [128, 512], F32) for i in range(2)]
                ts_ = [sbt(sm, "ts%d" % i, [128, 512], F32) for i in range(2)]
                tl_ = [sbt(sm, "tl%d" % i, [128, 512], F32) for i in range(2)]
                tgb = [Buf("tg%d" % i) for i in range(2)]
                tsb = [Buf("ts%d" % i) for i in range(2)]
                tlb = [Buf("tl%d" % i) for i in range(2)]
                ty_ = [sbt(sm, "ty%d" % i, [128, 512], F32) for i in range(2)]
                tyb = [Buf("ty%d" % i) for i in range(2)]
                ycnt = [0]

                def slab_jobs(e):
                    s = e % 2
                    jobs = []
                    for kc in range(8):
                        for cg in range(2):
                            jobs.append((wg[s][:, kc, cg * 512:(cg + 1) * 512], wug_d[e, kc * 128:(kc + 1) * 128, cg * 512:(cg + 1) * 512], wgb[s], None))
                            jobs.append((wl[s][:, kc, cg * 512:(cg + 1) * 512], wul_d[e, kc * 128:(kc + 1) * 128, cg * 512:(cg + 1) * 512], wlb[s], None))
                    for kc in range(8):
                        for cg in range(2):
                            jobs.append((wd[s][:, kc, cg * 512:(cg + 1) * 512], wdn_d[e, kc * 128:(kc + 1) * 128, cg * 512:(cg + 1) * 512], wdb[s],
                                         g2bc[:, cg * 512:(cg + 1) * 512]))
                    return jobs

                def run_jobs(jobs):
                    for (dst, src, dbuf, mul) in jobs:
                        if mul is None:
                            load_cast(dst, src, [dbuf], "act")
                        else:
                            load_cast(dst, src, [dbuf], "pool", mul_ap=mul, mul_bufs=[g2b])

                run_jobs(slab_jobs(0))
                stages = [(e, tg) for e in range(nexp) for tg in range(2)]
                cnt = [0]

                def up_chunk(e, tg, hc):
                    s = e % 2
                    a = act[tg]
                    u = cnt[0] % 2
                    cnt[0] += 1
                    pG, pGb = bank(2 * u)
                    pL, pLb = bank(2 * u + 1)

                    def mmu(pa, w):
                        def fn(en):
                            for kc in range(8):
                                ins = en.matmul(pa, lhsT=w[:, kc, hc * 128:(hc + 1) * 128], rhs=hT[:, kc, tg * 512:(tg + 1) * 512],
                                                start=(kc == 0), stop=(kc == 7))
                            return ins
                        return fn
                    fG, fL = mmu(pG, wg[s]), mmu(pL, wl[s])

                    def fGL(en):
                        fG(en)
                        return fL(en)
                    kb.op("pe", fGL, reads=hTb[tg * 4:(tg + 1) * 4] + [wgb[s], wlb[s]], writes=[pGb, pLb])
                    kb.op("dve", lambda en: en.tensor_scalar(out=full(tg_[u]), in0=pG, scalar1=bug[:, e, hc:hc + 1], scalar2=7.0,
                                                            op0=ALU.add, op1=ALU.min), reads=[pGb, bugb], writes=[tgb[u]])
                    kb.op("act", lambda en: en.activation(out=full(ts_[u]), in_=full(tg_[u]), func=AF.Sigmoid, scale=1.702),
                          reads=[tgb[u]], writes=[tsb[u]])
                    kb.op("dve", lambda en: en.tensor_scalar(out=full(tl_[u]), in0=pL, scalar1=bul[:, e, hc:hc + 1], scalar2=-6.0,
                                                            op0=ALU.add, op1=ALU.max), reads=[pLb, bulb], writes=[tlb[u]])
                    kb.op("dve", lambda en: en.tensor_tensor(out=full(ts_[u]), in0=full(tg_[u]), in1=full(ts_[u]), op=ALU.mult),
                          reads=[tgb[u], tsb[u]], writes=[tsb[u]])
                    kb.op("dve", lambda en: en.scalar_tensor_tensor(out=a[:, hc, :], in0=full(tl_[u]), scalar=8.0, in1=full(ts_[u]),
                                                                   op0=ALU.min, op1=ALU.mult),
                          reads=[tlb[u], tsb[u]], writes=[actb[tg]])

                def down(e, tg):
                    s = e % 2
                    a = act[tg]
                    for ti in range(4):
                        i = tg * 4 + ti
                        for hf in range(2):
                            pY, pYb = bank(4 + (2 * ti + hf) % 4)

                            def fn(en, pY=pY, hf=hf):
                                for kc in range(8):
                                    ins = en.matmul(pY, lhsT=a[:, kc, ti * 128:(ti + 1) * 128], rhs=wd[s][:, kc, hf * 512:(hf + 1) * 512],
                                                    start=(kc == 0), stop=(kc == 7))
                                return ins
                            kb.op("pe", fn, reads=[actb[tg], wdb[s]], writes=[pYb])
                            yv = ycnt[0] % 2
                            ycnt[0] += 1
                            kb.op("act", lambda en, pY=pY, yv=yv: en.activation(out=full(ty_[yv]), in_=pY, func=AF.Identity,
                                                                             scale=gates[:, i, e:e + 1]),
                                  reads=[pYb, gatesb[i]], writes=[tyb[yv]])
                            kb.op("pool", lambda en, hf=hf, yv=yv: en.tensor_tensor(
                                out=xres[:, i, hf * 512:(hf + 1) * 512], in0=full(ty_[yv]),
                                in1=xres[:, i, hf * 512:(hf + 1) * 512], op=ALU.add),
                                reads=[tyb[yv], xb[i]], writes=[xb[i]])

                up_chunk(0, 0, 0)
                up_chunk(0, 0, 1)
                for si, (e, tg) in enumerate(stages):
                    nxt = slab_jobs(e + 1) if e + 1 < nexp else []
                    per = (len(nxt) + 9) // 10
                    for hc in range(2, 8):
                        up_chunk(e, tg, hc)
                        k0 = (tg * 6 + hc - 2) * per
                        run_jobs(nxt[k0:k0 + per])
                    if si + 1 < len(stages):
                        up_chunk(stages[si + 1][0], stages[si + 1][1], 0)
                        up_chunk(stages[si + 1][0], stages[si + 1][1], 1)
                    down(e, tg)
                for i in range(NT):
                    kb.dma(out_d[b, t0 + i * 128:t0 + (i + 1) * 128, :], xres[:, i, :], reads=[xb[i]], sb=xb[i])
                kb.barrier()
        kb.barrier()
    return nc


def _prep_inputs(inputs):
    f = lambda a: np.ascontiguousarray(np.asarray(a, dtype=np.float32))
    x = f(inputs["x"])
    c = f(inputs["c"])
    pos = np.ascontiguousarray(np.asarray(inputs["positions"], dtype=np.int32))
    b_ada = f(inputs["b_ada"])[0]
    w_up = f(inputs["w_up"])[0]
    b_up = f(inputs["b_up"])[0]
    shared = {
        "w_ada": f(inputs["w_ada"])[0],
        "b_adaT": np.ascontiguousarray(b_ada.reshape(48, 128).T),
        "b_g1": np.ascontiguousarray(np.broadcast_to(b_ada[2048:3072], (128, D))),
        "b_g2": np.ascontiguousarray(np.broadcast_to(b_ada[5120:6144], (128, D))),
        "n1gT": np.ascontiguousarray(f(inputs["norm1_gain"])[0].reshape(8, 128).T),
        "n2gT": np.ascontiguousarray(f(inputs["norm2_gain"])[0].reshape(8, 128).T),
        "w_in": f(inputs["w_in"])[0],
        "lblT": np.ascontiguousarray(f(inputs["lower_bound_logits"]).reshape(2, 4, 128).transpose(2, 0, 1)),
        "hgainT": np.ascontiguousarray(f(inputs["hg_norm_gain"])[0].reshape(128, 1)),
        "w_hg": f(inputs["w_hg_branch"])[0],
        "w_attn": f(inputs["w_attn_branch"])[0],
        "w_out": f(inputs["w_out"])[0],
        "qg_bc": np.ascontiguousarray(np.broadcast_to(np.tile(f(inputs["q_norm_gain"])[0], 8), (128, 512))),
        "kg_bc": np.ascontiguousarray(np.broadcast_to(np.tile(f(inputs["k_norm_gain"])[0], 2), (128, 128))),
        "sink_bc": np.ascontiguousarray(np.broadcast_to(f(inputs["attn_sinks"])[0], (128, 8))),
        "invf_bc": np.ascontiguousarray(np.broadcast_to(
            (500000.0 ** (-np.arange(8, dtype=np.float32) / 8)).astype(np.float32), (128, 8))),
        "w_router": f(inputs["w_router"])[0],
        "br_bc": np.ascontiguousarray(np.broadcast_to(f(inputs["b_router"])[0], (128, 32))),
        "w_up_g": np.ascontiguousarray(w_up[:, :, 0::2]),
        "w_up_l": np.ascontiguousarray(w_up[:, :, 1::2]),
        "w_down": f(inputs["w_down"])[0],
        "bupgT": np.ascontiguousarray(b_up[:, 0::2].reshape(NEXP, 8, 128).transpose(2, 0, 1)),
        "buplT": np.ascontiguousarray(b_up[:, 1::2].reshape(NEXP, 8, 128).transpose(2, 0, 1)),
        "b_down": f(inputs["b_down"])[0],
        "ident": np.eye(128, dtype=np.float32),
    }
    j = np.arange(128)[:, None]
    i = np.arange(128)[None, :]
    shared["mask_bd"] = ((j <= i) & ((j // 64) == (i // 64))).astype(np.float32)
    shared["mask_cur"] = (j <= i).astype(np.float32)
    shared["mask_prev"] = (j > i).astype(np.float32)
    rm = np.ones((128, PASS_T), np.float32)
    rm[:, 0::64] = 0.0
    shared["resetmask"] = rm
    in_maps = []
    for cidx in range(NCORES):
        m = dict(shared)
        m["x"] = np.ascontiguousarray(x[2 * cidx:2 * cidx + 2])
        cc = c[2 * cidx:2 * cidx + 2]
        m["cT"] = np.ascontiguousarray(cc.reshape(2, 8, 128).transpose(2, 1, 0))
        pp = pos[2 * cidx:2 * cidx + 2]
        m["posT"] = np.ascontiguousarray(pp.reshape(2, 16, 128).transpose(2, 0, 1))
        in_maps.append(m)
    return in_maps


_LAST = {}


def kernel(**inputs):
    cfg = {}
    if os.environ.get("KDBG"):
        cfg = dict(dbg=True, npass=int(os.environ.get("KPASS", "1")), nexp=int(os.environ.get("KEXP", "2")),
                   stop=os.environ.get("KSTOP", ""))
    nc = build_program(cfg)
    in_maps = _prep_inputs(inputs)
    ne = cfg.get("nexp", NEXP)
    if ne != NEXP:
        for m in in_maps:
            for k in ("w_up_g", "w_up_l", "w_down"):
                m[k] = np.ascontiguousarray(m[k][:ne])
    res = run_bass_kernel_spmd(nc, in_maps, core_ids=list(range(NCORES)))
    _LAST["res"] = res
    out = np.concatenate([np.asarray(r["out"], dtype=np.float32) for r in res.results], axis=0)
    return out
```
